# Optimizing a Trainium2 kernel written in Bass

```python
import jax
import jax.numpy as jnp
from jax import lax
import numpy as np

D_MODEL = 2048
BATCH = 4
SEQ = 4096
DEPTH = 4

GRID_W = 64
CTX_LEN = 256
N_MIXERS = 4
N_MOD = 6
CHUNK = 64
SC_WIDTH = 3
GLA_HEADS = 4
GLA_DK = D_MODEL // (2 * GLA_HEADS)
GLA_DV = D_MODEL // GLA_HEADS
GLA_RANK = 16
GLA_NORMALIZER = 16.0
GLA_IN = 2 * GLA_HEADS * GLA_DK + 2 * GLA_HEADS * GLA_DV + 2 * GLA_RANK
LRU_WIDTH = D_MODEL
LRU_BLOCKS = 8
LRU_BW = LRU_WIDTH // LRU_BLOCKS
LRU_CONV = 4
RG_C = 8.0
HG_DIM = 128
HG_HEADS = D_MODEL // HG_DIM
N_EXPERTS = 32
TOP_K = 4
D_FF = 768
SWIGLU_LIMIT = 7.0
SWIGLU_ALPHA = 1.702
MOE_BLOCK = 128
LN_EPS = 1e-5
RMS_EPS = 1e-6
DN_ALPHA = (2 * DEPTH) ** 0.25
DN_BETA = (8 * DEPTH) ** -0.25
N_SC = len(range(0, DEPTH, N_MIXERS))
N_GLA = len(range(1, DEPTH, N_MIXERS))
N_LRU = len(range(2, DEPTH, N_MIXERS))
N_HG = len(range(3, DEPTH, N_MIXERS))

kernel_name = 'hybrid_interleaved_flow_backbone'

F32 = jnp.float32


def layer_norm(x, g, b):
    xf = x.astype(F32)
    mu = xf.mean(-1, keepdims=True)
    var = jnp.square(xf - mu).mean(-1, keepdims=True)
    return ((xf - mu) * lax.rsqrt(var + LN_EPS) * g + b).astype(x.dtype)


def rms_norm_gate(o, g, gain):
    B, L, H, dv = o.shape
    o = o * lax.rsqrt(jnp.mean(o * o, -1, keepdims=True) + RMS_EPS) * gain.astype(F32)
    return o.reshape(B, L, H * dv) * jax.nn.silu(g.astype(F32))


def dwconv(u, w, b=None):
    K = w.shape[0]
    left = (K - 1) // 2
    L = u.shape[-2]
    up = jnp.pad(u, [(0, 0)] * (u.ndim - 2) + [(left, K - 1 - left), (0, 0)])
    y = sum(up[..., j:j + L, :] * w[j] for j in range(K))
    return y if b is None else y + b


def grid_row_conv(u, w, b=None):
    B, L, C = u.shape
    rows = L // GRID_W
    return dwconv(u.reshape(B, rows, GRID_W, C), w, b).reshape(B, L, C)


def flip_seq(*ts):
    return tuple(jnp.flip(t, axis=1) for t in ts)


def chunked_gla(q, k, v, log_g, s0):
    B, L, H, _ = q.shape
    dv = v.shape[-1]
    n = L // CHUNK
    blocks = lambda t: jnp.moveaxis(t.reshape(B, n, CHUNK, H, t.shape[-1]), 1, 0)
    mask = jnp.tril(jnp.ones((CHUNK, CHUNK), bool))

    def step(S, inp):
        qc, kc, vc, gc = inp
        b = jnp.cumsum(gc, axis=1)
        qd = qc * jnp.exp(b)
        kd = kc * jnp.exp(-b)
        att = jnp.where(mask, jnp.einsum('bthk,bshk->bhts', qd, kd), 0.0)
        o = jnp.einsum('bhts,bshv->bthv', att, vc) + jnp.einsum('bthk,bhkv->bthv', qd, S)
        b_end = b[:, -1]
        S = S * jnp.exp(b_end)[..., None] + jnp.einsum(
            'bshk,bshv->bhkv', kc * jnp.exp(b_end[:, None] - b), vc)
        return S, o

    S, o = lax.scan(step, s0, (blocks(q), blocks(k), blocks(v), blocks(log_g)))
    return jnp.moveaxis(o, 0, 1).reshape(B, L, H, dv), S


def bidir_gla(ctx_f, ctx_b, lat_f, lat_b):
    B, _, H, dk = ctx_f[0].shape
    dv = ctx_f[2].shape[-1]
    s0 = jnp.zeros((B, H, dk, dv), F32)
    oc_f, s_f = chunked_gla(*ctx_f, s0)
    oc_b, s_b = chunked_gla(*flip_seq(*ctx_b), s0)
    ol_f, _ = chunked_gla(*lat_f, s_f)
    ol_b, _ = chunked_gla(*flip_seq(*lat_b), s_b)
    return oc_f + jnp.flip(oc_b, 1), ol_f + jnp.flip(ol_b, 1)


def linear_scan(a, u, h0):
    u = u.at[:, 0].add(a[:, 0] * h0)
    combine = lambda l, r: (l[0] * r[0], r[0] * l[1] + r[1])
    return lax.associative_scan(combine, (a, u), axis=1)[1]


def short_conv_mixer(hc, hl, w_in, w_conv, w_out, with_ctx):
    def mix(h, conv):
        bg, cg, v = jnp.split(h @ w_in, 3, axis=-1)
        return (bg * conv(cg * v, w_conv)) @ w_out
    yc = mix(hc, dwconv) if with_ctx else None
    return yc, mix(hl, grid_row_conv)


def gla_mixer(hc, hl, w_in, w_gate2, b_gate, norm_g, w_out, with_ctx):
    KD, VD = GLA_HEADS * GLA_DK, GLA_HEADS * GLA_DV
    splits = [KD, 2 * KD, 2 * KD + VD, 2 * KD + 2 * VD, 2 * KD + 2 * VD + GLA_RANK]

    def project(h):
        B, L, _ = h.shape
        q, k, v, g, rf, rb = jnp.split(h @ w_in, splits, axis=-1)
        heads = lambda t, d: t.reshape(B, L, GLA_HEADS, d).astype(F32)
        q = heads(q, GLA_DK) * GLA_DK ** -0.5
        k = heads(k, GLA_DK)
        v = heads(v, GLA_DV)
        log_gate = lambda r, d: jax.nn.log_sigmoid(
            (r @ w_gate2[d] + b_gate[d]).astype(F32)).reshape(B, L, GLA_HEADS, GLA_DK) / GLA_NORMALIZER
        return (q, k, v, log_gate(rf, 0)), (q, k, v, log_gate(rb, 1)), g

    cf, cb, gc = project(hc)
    lf, lb, gl = project(hl)
    oc, ol = bidir_gla(cf, cb, lf, lb)
    readout = lambda o, g, h: rms_norm_gate(o, g, norm_g).astype(h.dtype) @ w_out
    yc = readout(oc, gc, hc) if with_ctx else None
    return yc, readout(ol, gl, hl)


def rglru_mixer(hc, hl, w_in, w_conv, b_conv, w_gate, b_gate, lam, w_out, with_ctx):
    def branches(h, conv):
        B, L, _ = h.shape
        y, xb = jnp.split(h @ w_in, 2, axis=-1)
        xc = conv(xb, w_conv, b_conv)
        pre = jnp.einsum('blnc,dgncm->dgblnm', xc.reshape(B, L, LRU_BLOCKS, LRU_BW), w_gate)
        gates = jax.nn.sigmoid((pre.reshape(2, 2, B, L, LRU_WIDTH)
                                + b_gate[:, :, None, None, :]).astype(F32))
        log_a = RG_C * gates[:, 0] * jax.nn.log_sigmoid(lam.astype(F32))[:, None, None, :]
        u = jnp.sqrt(-jnp.expm1(2.0 * log_a)) * gates[:, 1] * xc.astype(F32)
        return jax.nn.gelu(y), jnp.exp(log_a), u

    yc, ac, uc = branches(hc, dwconv)
    yl, al, ul = branches(hl, grid_row_conv)
    zero = jnp.zeros((hc.shape[0], LRU_WIDTH), F32)
    hcf = linear_scan(ac[0], uc[0], zero)
    hcb = linear_scan(*flip_seq(ac[1], uc[1]), zero)
    hlf = linear_scan(al[0], ul[0], hcf[:, -1])
    hlb = linear_scan(*flip_seq(al[1], ul[1]), hcb[:, -1])
    readout = lambda r, y, h: (r.astype(h.dtype) * y) @ w_out
    out_c = readout(hcf + jnp.flip(hcb, 1), yc, hc) if with_ctx else None
    return out_c, readout(hlf + jnp.flip(hlb, 1), yl, hl)


def hgrn2_lower_bound(raw, layer):
    p = jax.nn.softmax(raw.astype(F32), axis=0)
    return jnp.cumsum(p, axis=0)[layer] - p[0]


def hgrn2_mixer(hc, hl, w_in, lb, norm_g, w_out, with_ctx):
    log_lb = jnp.log(lb).reshape(HG_HEADS, HG_DIM)
    log_1m_lb = jnp.log1p(-lb).reshape(HG_HEADS, HG_DIM)

    def project(h):
        B, L, _ = h.shape
        q, ff, fb, i, g = jnp.split(h @ w_in, 5, axis=-1)
        heads = lambda t: t.reshape(B, L, HG_HEADS, HG_DIM).astype(F32)
        q = jax.nn.silu(heads(q)) * HG_DIM ** -0.5
        v = heads(i)

        def direction(f):
            log_f = jnp.logaddexp(log_lb, log_1m_lb + jax.nn.log_sigmoid(heads(f)))
            return (q, -jnp.expm1(log_f), v, log_f)
        return direction(ff), direction(fb), g

    cf, cb, gc = project(hc)
    lf, lbk, gl = project(hl)
    oc, ol = bidir_gla(cf, cb, lf, lbk)
    readout = lambda o, g, h: rms_norm_gate(o, g, norm_g).astype(h.dtype) @ w_out
    yc = readout(oc, gc, hc) if with_ctx else None
    return yc, readout(ol, gl, hl)


def moe_ffn(h, w_router, b_router, w_gu, b_gu, w_down, b_down):
    N, D = h.shape
    logits = (h @ w_router + b_router).astype(F32)
    top_v, top_e = lax.top_k(logits, TOP_K)
    gate = jax.nn.softmax(top_v, axis=-1).reshape(-1)
    e_flat = top_e.reshape(-1)
    n_assign = N * TOP_K
    order = jnp.argsort(e_flat)
    e_sorted = e_flat[order]
    counts = jnp.bincount(e_flat, length=N_EXPERTS)
    padded = (counts + MOE_BLOCK - 1) // MOE_BLOCK * MOE_BLOCK
    pad_end = jnp.cumsum(padded)
    dest = ((pad_end - padded)[e_sorted] + jnp.arange(n_assign)
            - (jnp.cumsum(counts) - counts)[e_sorted])
    n_blocks = -(-(n_assign + N_EXPERTS * (MOE_BLOCK - 1)) // MOE_BLOCK)
    n_rows = n_blocks * MOE_BLOCK
    row_tok = jnp.full((n_rows,), N, jnp.int32).at[dest].set((order // TOP_K).astype(jnp.int32))
    row_gate = jnp.zeros((n_rows,), F32).at[dest].set(gate[order])
    block_e = jnp.minimum(
        jnp.searchsorted(pad_end, jnp.arange(n_blocks) * MOE_BLOCK, side='right'), N_EXPERTS - 1)
    h_pad = jnp.concatenate([h, jnp.zeros((1, D), h.dtype)], axis=0)

    def expert_block(args):
        rows, e = args
        g, u = jnp.split(h_pad[rows] @ w_gu[e] + b_gu[e], 2, axis=-1)
        g = jnp.minimum(g, SWIGLU_LIMIT)
        u = jnp.clip(u, -SWIGLU_LIMIT, SWIGLU_LIMIT)
        return (g * jax.nn.sigmoid(SWIGLU_ALPHA * g) * (u + 1.0)) @ w_down[e] + b_down[e]

    y = lax.map(expert_block, (row_tok.reshape(n_blocks, MOE_BLOCK), block_e))
    y = y.reshape(n_rows, D).astype(F32) * row_gate[:, None]
    return jax.ops.segment_sum(y, row_tok, num_segments=N + 1)[:N].astype(h.dtype)


def setup_inputs(seed: int = 0) -> dict:
    key = jax.random.key(seed)
    ks = iter(jax.random.split(key, 40))
    nrm = lambda shape, scale: jax.random.normal(next(ks), shape, F32) * scale
    D = D_MODEL
    KD, VD = GLA_HEADS * GLA_DK, GLA_HEADS * GLA_DV
    HW = HG_HEADS * HG_DIM
    u = jax.random.uniform(next(ks), (N_LRU, 2, LRU_WIDTH), F32, 0.9, 0.999)
    a = u ** (1.0 / RG_C)
    lru_lambda = jnp.log(a) - jnp.log1p(-a)
    return {
        'x': nrm((BATCH, SEQ, D), 1.0),
        'c': nrm((BATCH, D), 1.0),
        'ctx': nrm((BATCH, CTX_LEN, D), 1.0),
        'c_ctx': nrm((D,), 1.0),
        'ada_w': nrm((DEPTH, D, N_MOD * D), D ** -0.5),
        'ada_b': nrm((DEPTH, N_MOD * D), 0.02),
        'ln_g': 1.0 + nrm((DEPTH, 2, D), 0.02),
        'ln_b': nrm((DEPTH, 2, D), 0.02),
        'sc_w_in': nrm((N_SC, D, 3 * D), D ** -0.5),
        'sc_conv': nrm((N_SC, SC_WIDTH, D), SC_WIDTH ** -0.5),
        'sc_w_out': nrm((N_SC, D, D), D ** -0.5 * DN_BETA),
        'gla_w_in': nrm((N_GLA, D, GLA_IN), D ** -0.5),
        'gla_w_gate2': nrm((N_GLA, 2, GLA_RANK, KD), GLA_RANK ** -0.5),
        'gla_b_gate': nrm((N_GLA, 2, KD), 0.5),
        'gla_norm': 1.0 + nrm((N_GLA, GLA_DV), 0.02),
        'gla_w_out': nrm((N_GLA, VD, D), VD ** -0.5 * DN_BETA),
        'lru_w_in': nrm((N_LRU, D, 2 * LRU_WIDTH), D ** -0.5),
        'lru_conv': nrm((N_LRU, LRU_CONV, LRU_WIDTH), LRU_CONV ** -0.5),
        'lru_conv_b': nrm((N_LRU, LRU_WIDTH), 0.02),
        'lru_w_gate': nrm((N_LRU, 2, 2, LRU_BLOCKS, LRU_BW, LRU_BW), LRU_BW ** -0.5),
        'lru_b_gate': nrm((N_LRU, 2, 2, LRU_WIDTH), 0.02),
        'lru_lambda': lru_lambda,
        'lru_w_out': nrm((N_LRU, LRU_WIDTH, D), LRU_WIDTH ** -0.5 * DN_BETA),
        'hg_w_in': nrm((N_HG, D, 5 * HW), D ** -0.5),
        'hg_lb_raw': nrm((DEPTH, HW), 0.1),
        'hg_norm': 1.0 + nrm((N_HG, HG_DIM), 0.02),
        'hg_w_out': nrm((N_HG, HW, D), HW ** -0.5 * DN_BETA),
        'moe_w_router': nrm((DEPTH, D, N_EXPERTS), D ** -0.5),
        'moe_b_router': nrm((DEPTH, N_EXPERTS), 0.01),
        'moe_w_gu': nrm((DEPTH, N_EXPERTS, D, 2 * D_FF), D ** -0.5),
        'moe_b_gu': nrm((DEPTH, N_EXPERTS, 2 * D_FF), 0.02),
        'moe_w_down': nrm((DEPTH, N_EXPERTS, D_FF, D), D_FF ** -0.5 * DN_BETA),
        'moe_b_down': nrm((DEPTH, N_EXPERTS, D), 0.02),
    }


def reference(x, c, ctx, c_ctx, ada_w, ada_b, ln_g, ln_b, sc_w_in, sc_conv, sc_w_out,
              gla_w_in, gla_w_gate2, gla_b_gate, gla_norm, gla_w_out,
              lru_w_in, lru_conv, lru_conv_b, lru_w_gate, lru_b_gate, lru_lambda, lru_w_out,
              hg_w_in, hg_lb_raw, hg_norm, hg_w_out,
              moe_w_router, moe_b_router, moe_w_gu, moe_b_gu, moe_w_down, moe_b_down):
    xl, xc = x, ctx
    B, L, D = x.shape
    for i in range(DEPTH):
        kind, j = i % N_MIXERS, i // N_MIXERS
        with_ctx = i < DEPTH - 1
        mod_l = jnp.split((jax.nn.silu(c) @ ada_w[i] + ada_b[i])[:, None, :], N_MOD, axis=-1)
        mod_c = jnp.split(jax.nn.silu(c_ctx) @ ada_w[i] + ada_b[i], N_MOD, axis=-1)
        hl = xl * (1.0 + mod_l[1]) + mod_l[0]
        hc = xc * (1.0 + mod_c[1]) + mod_c[0]
        if kind == 0:
            yc, yl = short_conv_mixer(hc, hl, sc_w_in[j], sc_conv[j], sc_w_out[j], with_ctx)
        elif kind == 1:
            yc, yl = gla_mixer(hc, hl, gla_w_in[j], gla_w_gate2[j], gla_b_gate[j],
                               gla_norm[j], gla_w_out[j], with_ctx)
        elif kind == 2:
            yc, yl = rglru_mixer(hc, hl, lru_w_in[j], lru_conv[j], lru_conv_b[j], lru_w_gate[j],
                                 lru_b_gate[j], lru_lambda[j], lru_w_out[j], with_ctx)
        else:
            yc, yl = hgrn2_mixer(hc, hl, hg_w_in[j], hgrn2_lower_bound(hg_lb_raw, i),
                                 hg_norm[j], hg_w_out[j], with_ctx)
        xl = layer_norm(DN_ALPHA * xl + mod_l[2] * yl, ln_g[i, 0], ln_b[i, 0])
        hl = xl * (1.0 + mod_l[4]) + mod_l[3]
        moe_args = (moe_w_router[i], moe_b_router[i], moe_w_gu[i], moe_b_gu[i],
                    moe_w_down[i], moe_b_down[i])
        if with_ctx:
            xc = layer_norm(DN_ALPHA * xc + mod_c[2] * yc, ln_g[i, 0], ln_b[i, 0])
            hc = xc * (1.0 + mod_c[4]) + mod_c[3]
            n_ctx = hc.shape[0] * hc.shape[1]
            y = moe_ffn(jnp.concatenate([hc.reshape(-1, D), hl.reshape(-1, D)], axis=0), *moe_args)
            xc = layer_norm(DN_ALPHA * xc + mod_c[5] * y[:n_ctx].reshape(hc.shape),
                            ln_g[i, 1], ln_b[i, 1])
            yl = y[n_ctx:].reshape(B, L, D)
        else:
            yl = moe_ffn(hl.reshape(-1, D), *moe_args).reshape(B, L, D)
        xl = layer_norm(DN_ALPHA * xl + mod_l[5] * yl, ln_g[i, 1], ln_b[i, 1])
    return xl
```

```python
import contextlib
import numpy as np
import ml_dtypes
import concourse.bass as bass
import concourse.mybir as mybir
from concourse.bass_utils import run_bass_kernel_spmd

F32 = mybir.dt.float32
BF16 = mybir.dt.bfloat16
AF = mybir.ActivationFunctionType
ALU = mybir.AluOpType
AX = mybir.AxisListType

D = 2048
KC = 16
NE = 32
DFF = 768
DEPTH = 4
NTOK = 2176
DN_ALPHA = (2 * DEPTH) ** 0.25
LN_EPS = 1e-5
WINDOW = 3


class Buf:
    __slots__ = ("t", "lw", "rd", "name")

    def __init__(self, t, name=""):
        self.t = t
        self.lw = None
        self.rd = {}
        self.name = name

    def __getitem__(self, idx):
        return self.t[idx]


class _Eng:
    def __init__(self, name, eng, sem):
        self.name, self.eng, self.sem = name, eng, sem
        self.count = 0
        self.seen = {}


class _Q:
    def __init__(self, E, sems):
        self.E, self.sems = E, sems
        self.vals = [0] * len(sems)
        self.next = 0


class Prog:
    def __init__(self):
        self.nc = bass.Bass("TRN2", target_bir_lowering=False)
        self.es = contextlib.ExitStack()
        nc = self.nc
        self.E = {}
        for name, eng in (("pe", nc.tensor), ("act", nc.scalar), ("dve", nc.vector),
                          ("pool", nc.gpsimd), ("sp", nc.sync)):
            sem = self.es.enter_context(nc.semaphore("s_" + name))
            self.E[name] = _Eng(name, eng, sem)
        self.Q = {}
        for q, n in (("sp", 8), ("pool", 14), ("act", 4)):
            sems = [self.es.enter_context(nc.semaphore("d_%s%d" % (q, i))) for i in range(n)]
            self.Q[q] = _Q(self.E[q], sems)
        self.nbuf = 0

    def sb(self, shape, dtype, name=None):
        self.nbuf += 1
        name = "sb_" + (name or ("%d" % self.nbuf))
        return Buf(self.es.enter_context(self.nc.sbuf_tensor(name, list(shape), dtype)), name)

    def ps(self, shape, dtype, name=None):
        self.nbuf += 1
        name = "ps_" + (name or ("%d" % self.nbuf))
        return Buf(self.es.enter_context(self.nc.psum_tensor(name, list(shape), dtype)), name)

    def din(self, name, shape, dtype):
        return self.nc.dram_tensor(name, list(shape), dtype, kind="ExternalInput").ap()

    def dout(self, name, shape, dtype):
        return self.nc.dram_tensor(name, list(shape), dtype, kind="ExternalOutput").ap()

    def _wait(self, E, sem, val):
        if E.seen.get(sem.num, 0) < val:
            E.eng.wait_ge(sem, val)
            E.seen[sem.num] = val

    def _sync(self, E, rd, wr, is_dma):
        evs = []
        for b in rd:
            if b.lw is not None:
                evs.append(b.lw)
        for b in wr:
            if b.lw is not None:
                evs.append(b.lw)
            evs.extend(b.rd.values())
        for (sem, val, src) in evs:
            if src == E.name and not is_dma:
                if src == "pe" or val <= E.count - WINDOW:
                    continue
            self._wait(E, sem, val)

    def op(self, en, fn, rd=(), wr=()):
        E = self.E[en]
        self._sync(E, rd, wr, False)
        ins = fn(E.eng)
        E.count += 1
        ins.then_inc(E.sem, 1)
        ev = (E.sem, E.count, en)
        for b in rd:
            b.rd[en] = ev
        for b in wr:
            b.lw = ev
            b.rd = {}
        return ins

    def dma(self, q, out, in_, rd=(), wr=()):
        Q = self.Q[q]
        E = Q.E
        k = Q.next
        Q.next = (k + 1) % len(Q.sems)
        sem = Q.sems[k]
        if Q.vals[k] > 0:
            self._wait(E, sem, Q.vals[k])
        self._sync(E, rd, wr, True)
        ins = E.eng.dma_start(out=out, in_=in_)
        ins.then_inc(sem, 16)
        Q.vals[k] += 16
        ev = (sem, Q.vals[k], "dma_" + q + str(k))
        for b in rd:
            b.rd["dma_" + q + str(k)] = ev
        for b in wr:
            b.lw = ev
            b.rd = {}

    def finish(self):
        for q, Q in self.Q.items():
            for k, sem in enumerate(Q.sems):
                if Q.vals[k] > 0:
                    self._wait(Q.E, sem, Q.vals[k])
        self.es.close()
        return self.nc


def relayout_kn(W, cols=256):
    K, N = W.shape
    ns = N // cols
    return np.ascontiguousarray(W.reshape(K // 128, 128, ns, cols).transpose(2, 1, 0, 3)).reshape(ns, 128, (K // 128) * cols)


def relayout_gu(W):
    E = W.shape[0]
    return np.ascontiguousarray(W.reshape(E, 16, 128, 2, 6, 128).transpose(0, 4, 2, 1, 3, 5)).reshape(E, 6, 128, 16 * 256)


def vec_pk(v):
    v = np.asarray(v)
    lead = v.shape[:-1]
    a = v.reshape(*lead, 16, 128)
    a = np.moveaxis(a, -1, 0)
    return np.ascontiguousarray(a)


def make_consts(P):
    nc = P.nc
    ident = P.sb([128, 128], F32, "ident")
    P.op("pool", lambda e: e.memset(ident[:], 0.0), wr=[ident])
    P.op("pool", lambda e: e.affine_select(out=ident[:], in_=ident[:], pattern=[[-1, 128]],
                                           compare_op=ALU.not_equal, fill=1.0, base=0,
                                           channel_multiplier=1), rd=[ident], wr=[ident])
    identb = P.sb([128, 128], BF16, "identb")
    P.op("dve", lambda e: e.tensor_copy(out=identb[:], in_=ident[:]), rd=[ident], wr=[identb])
    ones = P.sb([128, 128], F32, "ones")
    P.op("dve", lambda e: e.memset(ones[:], 1.0 / D), wr=[ones])
    return ident, identb, ones


def compute_mods(P, cv_d, adaw_d, adab_d, nmod, ring, psum):
    cv = P.sb([128, KC, 2], F32, "cv")
    P.dma("sp", cv[:], cv_d[:, :, :], wr=[cv])
    scv = P.sb([128, KC, 2], BF16, "scv")
    P.op("act", lambda e: e.activation(out=scv[:], in_=cv[:], func=AF.Silu), rd=[cv], wr=[scv])
    adab = P.sb([128, nmod, KC], F32, "adab")
    P.dma("sp", adab[:], adab_d[:, :, :], wr=[adab])
    mods = P.sb([128, nmod, KC, 2], F32, "mods")
    for s in range(nmod * 8):
        slot = ring[s % len(ring)]
        P.dma("pool", slot[:], adaw_d[s].rearrange("p (k c) -> p k c", k=KC), wr=[slot])
        m, r = divmod(s, 8)
        for h in range(2):
            kk = r * 2 + h
            for k in range(KC):
                P.op("pe", lambda e: e.matmul(psum[:, 0:2], lhsT=slot[:, k, h * 128:(h + 1) * 128], rhs=scv[:, k, :],
                                               start=(k == 0), stop=(k == KC - 1)), rd=[slot, scv], wr=[psum])
            P.op("dve", lambda e: e.tensor_scalar(out=mods[:, m, kk, :], in0=psum[:, 0:2], scalar1=adab[:, m, kk:kk + 1],
                                                  scalar2=None, op0=ALU.add), rd=[psum, adab], wr=[mods])
    return mods


BLOCKS = [(0, [(0, 256), (256, 512)]), (768, [(0, 384), (384, 384)]), (1536, [(0, 384), (384, 256)])]


def build_phase_b(kind=0, n_experts=NE, blocks=BLOCKS):
    P = Prog()
    nc = P.nc
    xT_d = P.din("xT", [D, NTOK], F32)
    if kind == 0:
        oT_d = P.din("oT", [D, NTOK], BF16)
    else:
        oF_d = P.din("oF", [D, NTOK], F32)
        oB_d = P.din("oB", [D, NTOK], F32)
        gG_d = P.din("gG", [D, NTOK], F32)
        if kind != 2:
            hc = 4 if kind == 1 else 1
            gain_d = P.din("gain", [128, hc], F32)
    cv_d = P.din("cv", [128, KC, 2], F32)
    adaw_d = P.din("adaw", [32, 128, KC * 256], F32)
    adab_d = P.din("adab", [128, 4, KC], F32)
    lng_d = P.din("lng", [128, 2, KC], F32)
    lnb_d = P.din("lnb", [128, 2, KC], F32)
    wo_d = P.din("wo", [8, 128, KC * 256], F32)
    wr_d = P.din("wr", [128, KC, NE], F32)
    br_d = P.din("br", [128, NE], F32)
    wgu_d = P.din("wgu", [NE, 6, 128, KC * 256], F32)
    wd_d = P.din("wd", [NE, DFF, D], F32)
    bgu_d = P.din("bgu", [128, NE, 12], F32)
    bdn_d = P.din("bdn", [NE, D], F32)
    sel_d = P.din("sel", [64, NE * 128], BF16)
    out_d = P.dout("xo", [D, NTOK], F32)

    ident, identb, ones = make_consts(P)
    pb = [P.ps([128, 512], F32, "pb%d" % i) for i in range(7)]
    pbt = P.ps([128, 512], BF16, "pbt")
    gu_ring = [P.sb([128, KC, 256], BF16, "gur%d" % i) for i in range(4)]
    d_ring = [P.sb([128, D], BF16, "dr%d" % i) for i in range(8)]

    lng = P.sb([128, 2, KC], F32, "lng"); P.dma("sp", lng[:], lng_d[:, :, :], wr=[lng])
    lnb = P.sb([128, 2, KC], F32, "lnb"); P.dma("sp", lnb[:], lnb_d[:, :, :], wr=[lnb])
    wr = P.sb([128, KC, NE], F32, "wr"); P.dma("sp", wr[:], wr_d[:, :, :], wr=[wr])
    brt = P.sb([128, NE], F32, "brt"); P.dma("sp", brt[:], br_d[:, :], wr=[brt])
    bgu = P.sb([128, NE, 12], F32, "bgu"); P.dma("sp", bgu[:], bgu_d[:, :, :], wr=[bgu])
    bdn = P.sb([NE, D], F32, "bdn"); P.dma("sp", bdn[:], bdn_d[:, :], wr=[bdn])
    sel = P.sb([64, NE * 128], BF16, "sel"); P.dma("sp", sel[:], sel_d[:, :], wr=[sel])

    mods = compute_mods(P, cv_d, adaw_d, adab_d, 4, gu_ring, pb[0])
    if kind in (1, 3):
        gain = P.sb([128, hc], F32, "gain"); P.dma("sp", gain[:], gain_d[:, :], wr=[gain])
        onesH = P.sb([128, 128], F32, "onesH")
        P.op("dve", lambda e: e.memset(onesH[:], 1.0 / (hc * 128)), wr=[onesH])
    HS = P.sb([128, KC, 2], F32, "HS"); HB = P.sb([128, KC, 2], F32, "HB")
    ZS = P.sb([128, KC], F32, "ZS"); ZB = P.sb([128, KC], F32, "ZB")
    for v in range(2):
        P.op("dve", lambda e: e.scalar_tensor_tensor(out=HS[:, :, v], in0=mods[:, 2, :, v], scalar=1.0, in1=lng[:, 0, :],
                                                     op0=ALU.add, op1=ALU.mult), rd=[mods, lng], wr=[HS])
        P.op("dve", lambda e: e.scalar_tensor_tensor(out=HB[:, :, v], in0=mods[:, 2, :, v], scalar=1.0, in1=lnb[:, 0, :],
                                                     op0=ALU.add, op1=ALU.mult), rd=[mods, lnb], wr=[HB])
        P.op("dve", lambda e: e.tensor_tensor(out=HB[:, :, v], in0=HB[:, :, v], in1=mods[:, 1, :, v], op=ALU.add),
             rd=[HB, mods], wr=[HB])
    P.op("dve", lambda e: e.tensor_scalar(out=ZS[:], in0=lng[:, 0, :], scalar1=float(DN_ALPHA), scalar2=None, op0=ALU.mult),
         rd=[lng], wr=[ZS])
    P.op("dve", lambda e: e.tensor_scalar(out=ZB[:], in0=lnb[:, 0, :], scalar1=float(DN_ALPHA), scalar2=None, op0=ALU.mult),
         rd=[lnb], wr=[ZB])

    Z = P.sb([128, KC, 768], F32, "Z")
    H = P.sb([128, KC, 768], BF16, "H")
    A = P.sb([128, 6, 768], BF16, "A")
    Hf = P.sb([128, KC, 128], F32, "Hf")
    gbc = [P.sb([128, 768], F32, "gbc%d" % i) for i in range(2)]
    GTf = P.sb([NE, 768], F32, "GTf")
    GT2 = P.sb([64, 768], BF16, "GT2")
    sq = [P.sb([128, 512], F32, "sq%d" % i) for i in range(2)]
    rstd = P.sb([128, 512], F32, "rstd")
    xn = [P.sb([128, 512], F32, "xn%d" % i) for i in range(2)]
    t1 = P.sb([128, 512], F32, "t1"); t2 = P.sb([128, 512], F32, "t2"); t3 = P.sb([128, 512], F32, "t3")
    lg = P.sb([128, NE], F32, "lg"); mx8 = P.sb([128, 8], F32, "mx8"); msk = P.sb([128, NE], F32, "msk")
    nm = P.sb([128, 1], F32, "nm"); ex = P.sb([128, NE], F32, "ex"); ssum = P.sb([128, 1], F32, "ssum")
    gate = P.sb([128, NE], F32, "gate"); gs = P.sb([128, NE], F32, "gs"); G2b = P.sb([128, 64], BF16, "G2b")

    def ln_stats(cols0, T):
        pm, pv = pb[0], pb[1]
        for k in range(KC):
            P.op("pe", lambda e: e.matmul(pm[:, :T], lhsT=ones[:], rhs=Z[:, k, cols0:cols0 + T], start=(k == 0), stop=(k == KC - 1)),
                 rd=[ones, Z], wr=[pm])
        for k in range(KC):
            P.op("dve", lambda e: e.tensor_tensor(out=Z[:, k, cols0:cols0 + T], in0=Z[:, k, cols0:cols0 + T], in1=pm[:, :T],
                                                  op=ALU.subtract), rd=[Z, pm], wr=[Z])
        for k in range(KC):
            s = sq[k % 2]
            P.op("act", lambda e: e.activation(out=s[:, :T], in_=Z[:, k, cols0:cols0 + T], func=AF.Square), rd=[Z], wr=[s])
            P.op("pe", lambda e: e.matmul(pv[:, :T], lhsT=ones[:], rhs=s[:, :T], start=(k == 0), stop=(k == KC - 1)),
                 rd=[ones, s], wr=[pv])
        s = sq[0]
        P.op("act", lambda e: e.activation(out=s[:, :T], in_=pv[:, :T], func=AF.Sqrt, bias=float(LN_EPS)), rd=[pv], wr=[s])
        P.op("dve", lambda e: e.reciprocal(out=rstd[:, :T], in_=s[:, :T]), rd=[s], wr=[rstd])

    for (tok0, groups) in blocks:
        ntok_b = sum(g[1] for g in groups)
        if kind == 0:
            P.dma("sp", H[:, :, :ntok_b], oT_d[:, tok0:tok0 + ntok_b].rearrange("(k p) t -> p k t", p=128), wr=[H])
        elif kind == 2:
            for (c0, T) in groups:
                g0 = tok0 + c0
                for k in range(KC):
                    rows = slice(k * 128, (k + 1) * 128)
                    P.dma("sp", t1[:, :T], oF_d[rows, g0:g0 + T], wr=[t1])
                    P.dma("sp", t2[:, :T], oB_d[rows, g0:g0 + T], wr=[t2])
                    P.dma("sp", t3[:, :T], gG_d[rows, g0:g0 + T], wr=[t3])
                    P.op("dve", lambda e: e.tensor_tensor(out=t1[:, :T], in0=t1[:, :T], in1=t2[:, :T], op=ALU.add), rd=[t1, t2], wr=[t1])
                    P.op("dve", lambda e: e.tensor_tensor(out=H[:, k, c0:c0 + T], in0=t1[:, :T], in1=t3[:, :T], op=ALU.mult), rd=[t1, t3], wr=[H])
        else:
            for (c0, T) in groups:
                g0 = tok0 + c0
                for hd in range(KC // hc):
                    pv = pb[1]
                    for ci in range(hc):
                        k = hd * hc + ci
                        rows = slice(k * 128, (k + 1) * 128)
                        P.dma("sp", t1[:, :T], oF_d[rows, g0:g0 + T], wr=[t1])
                        P.dma("sp", t2[:, :T], oB_d[rows, g0:g0 + T], wr=[t2])
                        P.op("dve", lambda e: e.tensor_tensor(out=Z[:, k, c0:c0 + T], in0=t1[:, :T], in1=t2[:, :T], op=ALU.add), rd=[t1, t2], wr=[Z])
                        s_ = sq[k % 2]
                        P.op("act", lambda e: e.activation(out=s_[:, :T], in_=Z[:, k, c0:c0 + T], func=AF.Square), rd=[Z], wr=[s_])
                        P.op("pe", lambda e: e.matmul(pv[:, :T], lhsT=onesH[:], rhs=s_[:, :T], start=(ci == 0), stop=(ci == hc - 1)),
                             rd=[onesH, s_], wr=[pv])
                    s_ = sq[0]
                    P.op("act", lambda e: e.activation(out=s_[:, :T], in_=pv[:, :T], func=AF.Sqrt, bias=1e-6), rd=[pv], wr=[s_])
                    P.op("dve", lambda e: e.reciprocal(out=rstd[:, :T], in_=s_[:, :T]), rd=[s_], wr=[rstd])
                    for ci in range(hc):
                        k = hd * hc + ci
                        rows = slice(k * 128, (k + 1) * 128)
                        P.dma("sp", t3[:, :T], gG_d[rows, g0:g0 + T], wr=[t3])
                        P.op("act", lambda e: e.activation(out=t2[:, :T], in_=t3[:, :T], func=AF.Silu), rd=[t3], wr=[t2])
                        P.op("dve", lambda e: e.tensor_tensor(out=t1[:, :T], in0=Z[:, k, c0:c0 + T], in1=rstd[:, :T], op=ALU.mult), rd=[Z, rstd], wr=[t1])
                        P.op("dve", lambda e: e.scalar_tensor_tensor(out=H[:, k, c0:c0 + T], in0=t1[:, :T], scalar=gain[:, ci:ci + 1], in1=t2[:, :T],
                                                                     op0=ALU.mult, op1=ALU.mult), rd=[t1, gain, t2], wr=[H])
        P.dma("sp", Z[:, :, :ntok_b], xT_d[:, tok0:tok0 + ntok_b].rearrange("(k p) t -> p k t", p=128), wr=[Z])
        for k in range(KC):
            P.op("act", lambda e: e.mul(out=Z[:, k, :ntok_b], in_=Z[:, k, :ntok_b], mul=float(DN_ALPHA)), rd=[Z], wr=[Z])
        for s in range(8):
            slot = gu_ring[s % 4]
            P.dma("pool", slot[:], wo_d[s].rearrange("p (k c) -> p k c", k=KC), wr=[slot])
            for (c0, T) in groups:
                v = 0 if (tok0 + c0) < 256 else 1
                for h in range(2):
                    d = s * 2 + h
                    pd = pb[2 + (d % 2)]
                    for k in range(KC):
                        P.op("pe", lambda e: e.matmul(pd[:, :T], lhsT=slot[:, k, h * 128:(h + 1) * 128], rhs=H[:, k, c0:c0 + T],
                                                       start=(k == 0), stop=(k == KC - 1)), rd=[slot, H], wr=[pd])
                    P.op("dve", lambda e: e.scalar_tensor_tensor(out=Z[:, d, c0:c0 + T], in0=pd[:, :T], scalar=mods[:, 0, d, v:v + 1],
                                                                 in1=Z[:, d, c0:c0 + T], op0=ALU.mult, op1=ALU.add),
                         rd=[pd, mods, Z], wr=[Z])
        for (c0, T) in groups:
            v = 0 if (tok0 + c0) < 256 else 1
            ln_stats(c0, T)
            for tt in range(T // 128):
                a0 = c0 + tt * 128
                for k in range(KC):
                    x_ = xn[k % 2]
                    P.op("dve", lambda e: e.tensor_tensor(out=x_[:, :128], in0=Z[:, k, a0:a0 + 128], in1=rstd[:, tt * 128:(tt + 1) * 128],
                                                          op=ALU.mult), rd=[Z, rstd], wr=[x_])
                    P.op("act", lambda e: e.activation(out=Hf[:, k, :], in_=x_[:, :128], func=AF.Identity,
                                                       bias=HB[:, k, v:v + 1], scale=HS[:, k, v:v + 1]), rd=[x_, HB, HS], wr=[Hf])
                    P.op("dve", lambda e: e.tensor_scalar(out=Z[:, k, a0:a0 + 128], in0=x_[:, :128], scalar1=ZS[:, k:k + 1],
                                                          scalar2=ZB[:, k:k + 1], op0=ALU.mult, op1=ALU.add), rd=[x_, ZS, ZB], wr=[Z])
                    P.op("pool", lambda e: e.tensor_copy(out=H[:, k, a0:a0 + 128], in_=Hf[:, k, :]), rd=[Hf], wr=[H])
                pl = pb[4]
                for k in range(KC):
                    P.op("pe", lambda e: e.matmul(pl[:, 0:NE], lhsT=Hf[:, k, :], rhs=wr[:, k, :], start=(k == 0), stop=(k == KC - 1)),
                         rd=[Hf, wr], wr=[pl])
                P.op("dve", lambda e: e.tensor_tensor(out=lg[:], in0=pl[:, 0:NE], in1=brt[:], op=ALU.add), rd=[pl, brt], wr=[lg])
                P.op("dve", lambda e: e.max(out=mx8[:], in_=lg[:]), rd=[lg], wr=[mx8])
                P.op("dve", lambda e: e.tensor_scalar(out=msk[:], in0=lg[:], scalar1=mx8[:, 3:4], scalar2=None, op0=ALU.is_ge),
                     rd=[lg, mx8], wr=[msk])
                P.op("dve", lambda e: e.tensor_scalar(out=nm[:], in0=mx8[:, 0:1], scalar1=-1.0, scalar2=None, op0=ALU.mult),
                     rd=[mx8], wr=[nm])
                P.op("act", lambda e: e.activation(out=ex[:], in_=lg[:], func=AF.Exp, bias=nm[:, 0:1], scale=1.0), rd=[lg, nm], wr=[ex])
                P.op("dve", lambda e: e.tensor_tensor(out=ex[:], in0=ex[:], in1=msk[:], op=ALU.mult), rd=[ex, msk], wr=[ex])
                P.op("dve", lambda e: e.reduce_sum(out=ssum[:], in_=ex[:], axis=AX.X), rd=[ex], wr=[ssum])
                P.op("dve", lambda e: e.reciprocal(out=ssum[:], in_=ssum[:]), rd=[ssum], wr=[ssum])
                P.op("dve", lambda e: e.tensor_scalar(out=gate[:], in0=ex[:], scalar1=ssum[:, 0:1], scalar2=None, op0=ALU.mult),
                     rd=[ex, ssum], wr=[gate])
                pt = pb[5]
                P.op("pe", lambda e: e.transpose(out=pt[0:NE, 0:128], in_=gate[:], identity=ident[:]), rd=[gate, ident], wr=[pt])
                P.op("act", lambda e: e.copy(out=GTf[:, a0:a0 + 128], in_=pt[0:NE, 0:128]), rd=[pt], wr=[GTf])
                P.op("dve", lambda e: e.tensor_scalar(out=gs[:], in0=gate[:], scalar1=float(1.0 / 1.702), scalar2=None, op0=ALU.mult),
                     rd=[gate], wr=[gs])
                P.op("dve", lambda e: e.tensor_copy(out=G2b[:, 0:NE], in_=gs[:]), rd=[gs], wr=[G2b])
                P.op("dve", lambda e: e.tensor_tensor(out=gs[:], in0=gs[:], in1=G2b[:, 0:NE], op=ALU.subtract), rd=[gs, G2b], wr=[gs])
                P.op("dve", lambda e: e.tensor_copy(out=G2b[:, NE:2 * NE], in_=gs[:]), rd=[gs], wr=[G2b])
                P.op("pe", lambda e: e.transpose(out=pbt[0:64, 0:128], in_=G2b[:], identity=identb[:]), rd=[G2b, identb], wr=[pbt])
                P.op("act", lambda e: e.copy(out=GT2[:, a0:a0 + 128], in_=pbt[0:64, 0:128]), rd=[pbt], wr=[GT2])
        for (c0, T) in groups:
            v = 0 if (tok0 + c0) < 256 else 1
            for d in range(KC):
                pd = pb[2 + (d % 2)]
                P.op("pe", lambda e: e.matmul(pd[:, :T], lhsT=bdn[:, d * 128:(d + 1) * 128], rhs=GTf[:, c0:c0 + T], start=True, stop=True),
                     rd=[bdn, GTf], wr=[pd])
                P.op("dve", lambda e: e.scalar_tensor_tensor(out=Z[:, d, c0:c0 + T], in0=pd[:, :T], scalar=mods[:, 3, d, v:v + 1],
                                                             in1=Z[:, d, c0:c0 + T], op0=ALU.mult, op1=ALU.add), rd=[pd, mods, Z], wr=[Z])
        units = []
        for e_ in range(n_experts):
            for j in range(6):
                units.append(("gu", e_, j))
            for j in range(6):
                units.append(("d", e_, j))
        state = {"issued": 0, "gu_i": 0, "d_i": 0, "gu_done": 0, "d_done": 0, "slot": {}}

        def issue():
            while state["issued"] < len(units):
                kind, e_, j = units[state["issued"]]
                if kind == "gu":
                    if state["gu_i"] >= state["gu_done"] + 4:
                        break
                    slot = gu_ring[state["gu_i"] % 4]; state["gu_i"] += 1
                    P.dma("pool", slot[:], wgu_d[e_, j].rearrange("p (k c) -> p k c", k=KC), wr=[slot])
                else:
                    if state["d_i"] >= state["d_done"] + 8:
                        break
                    slot = d_ring[state["d_i"] % 8]; state["d_i"] += 1
                    P.dma("pool", slot[:], wd_d[e_, j * 128:(j + 1) * 128, :], wr=[slot])
                state["slot"][(kind, e_, j)] = slot
                state["issued"] += 1

        issue()
        ui = 0
        gi_ctr = 0
        for e_ in range(n_experts):
            gb = gbc[e_ % 2]
            for (c0, T) in groups:
                pg = pb[6]
                P.op("pe", lambda e: e.matmul(pg[:, :T], lhsT=sel[:, e_ * 128:(e_ + 1) * 128], rhs=GT2[:, c0:c0 + T], start=True, stop=True),
                     rd=[sel, GT2], wr=[pg])
                P.op("act", lambda e: e.copy(out=gb[:, c0:c0 + T], in_=pg[:, :T]), rd=[pg], wr=[gb])
            for j in range(6):
                slot = state["slot"][("gu", e_, j)]
                for (c0, T) in groups:
                    pg_, pu_ = (pb[0], pb[1]) if gi_ctr % 2 == 0 else (pb[4], pb[5])
                    gi_ctr += 1
                    for h, pp in ((0, pg_), (1, pu_)):
                        for k in range(KC):
                            P.op("pe", lambda e: e.matmul(pp[:, :T], lhsT=slot[:, k, h * 128:(h + 1) * 128], rhs=H[:, k, c0:c0 + T],
                                                           start=(k == 0), stop=(k == KC - 1)), rd=[slot, H], wr=[pp])
                    P.op("dve", lambda e: e.tensor_scalar(out=t1[:, :T], in0=pg_[:, :T], scalar1=bgu[:, e_, j:j + 1], scalar2=7.0,
                                                          op0=ALU.add, op1=ALU.min), rd=[pg_, bgu], wr=[t1])
                    P.op("act", lambda e: e.activation(out=t2[:, :T], in_=t1[:, :T], func=AF.Silu, scale=1.702), rd=[t1], wr=[t2])
                    P.op("dve", lambda e: e.tensor_scalar(out=t3[:, :T], in0=pu_[:, :T], scalar1=bgu[:, e_, 6 + j:7 + j], scalar2=7.0,
                                                          op0=ALU.add, op1=ALU.min), rd=[pu_, bgu], wr=[t3])
                    P.op("dve", lambda e: e.tensor_scalar(out=t3[:, :T], in0=t3[:, :T], scalar1=-7.0, scalar2=1.0,
                                                          op0=ALU.max, op1=ALU.add), rd=[t3], wr=[t3])
                    P.op("dve", lambda e: e.tensor_tensor(out=t3[:, :T], in0=t3[:, :T], in1=t2[:, :T], op=ALU.mult), rd=[t3, t2], wr=[t3])
                    P.op("dve", lambda e: e.tensor_tensor(out=A[:, j, c0:c0 + T], in0=t3[:, :T], in1=gb[:, c0:c0 + T], op=ALU.mult),
                         rd=[t3, gb], wr=[A])
                state["gu_done"] += 1
                issue()
            dslots = [state["slot"][("d", e_, j)] for j in range(6)]
            for (c0, T) in groups:
                v = 0 if (tok0 + c0) < 256 else 1
                for d in range(KC):
                    pd = pb[2 + (d % 2)]
                    for j in range(6):
                        P.op("pe", lambda e: e.matmul(pd[:, :T], lhsT=dslots[j][:, d * 128:(d + 1) * 128], rhs=A[:, j, c0:c0 + T],
                                                       start=(j == 0), stop=(j == 5)), rd=[dslots[j], A], wr=[pd])
                    P.op("dve", lambda e: e.scalar_tensor_tensor(out=Z[:, d, c0:c0 + T], in0=pd[:, :T], scalar=mods[:, 3, d, v:v + 1],
                                                                 in1=Z[:, d, c0:c0 + T], op0=ALU.mult, op1=ALU.add),
                         rd=[pd, mods, Z], wr=[Z])
            state["d_done"] += 6
            issue()
        for (c0, T) in groups:
            ln_stats(c0, T)
            for k in range(KC):
                x_ = xn[k % 2]
                P.op("dve", lambda e: e.tensor_tensor(out=x_[:, :T], in0=Z[:, k, c0:c0 + T], in1=rstd[:, :T], op=ALU.mult),
                     rd=[Z, rstd], wr=[x_])
                P.op("act", lambda e: e.activation(out=Z[:, k, c0:c0 + T], in_=x_[:, :T], func=AF.Identity,
                                                   bias=lnb[:, 1, k:k + 1], scale=lng[:, 1, k:k + 1]), rd=[x_, lnb, lng], wr=[Z])
            P.dma("sp", out_d[:, tok0 + c0:tok0 + c0 + T].rearrange("(k p) t -> p k t", p=128), Z[:, :, c0:c0 + T], rd=[Z])
    return P.finish()


def make_sel():
    s = np.zeros((64, NE * 128), dtype=np.float32)
    for e_ in range(NE):
        s[e_, e_ * 128:(e_ + 1) * 128] = 1.0
        s[32 + e_, e_ * 128:(e_ + 1) * 128] = 1.0
    return s.astype(ml_dtypes.bfloat16)


SEQ = [(0, 256)] + [(256 + 512 * i_, 512) for i_ in range(8)]
LSEQ = 4352


class WStream:
    def __init__(self, P, ring, srcs):
        self.P, self.ring, self.srcs = P, ring, srcs
        self.i = 0
        self.issued = 0

    def next(self):
        n = len(self.ring)
        while self.issued < len(self.srcs) and self.issued < self.i + n:
            slot = self.ring[self.issued % n]
            self.P.dma("pool", slot[:], self.srcs[self.issued], wr=[slot])
            self.issued += 1
        slot = self.ring[self.i % n]
        self.i += 1
        return slot


def conv_taps(P, y, p, cw, k, T, L):
    y3 = y[:, :T].rearrange("p (r l) -> p r l", l=L)
    p3 = p[:, :T].rearrange("p (r l) -> p r l", l=L)
    for o in (-2, -1, 1, 2):
        a, b = max(0, -o), L - max(0, o)
        P.op("dve", lambda e: e.scalar_tensor_tensor(out=y3[:, :, a:b], in0=p3[:, :, a + o:b + o], scalar=cw[:, k, o + 2:o + 3],
                                                     in1=y3[:, :, a:b], op0=ALU.mult, op1=ALU.add), rd=[p, cw, y], wr=[y])


def build_phase_a(kind):
    P = Prog()
    nc = P.nc
    xs_d = P.din("xs", [D, LSEQ], F32)
    cv_d = P.din("cv", [128, KC, 2], F32)
    adaw_d = P.din("adaw", [16, 128, KC * 256], F32)
    adab_d = P.din("adab", [128, 2, KC], F32)
    ident, identb, ones = make_consts(P)
    pb = [P.ps([128, 512], F32, "pb%d" % i) for i in range(8)]
    mring = [P.sb([128, KC, 256], BF16, "mring%d" % i) for i in range(2)]
    mods = compute_mods(P, cv_d, adaw_d, adab_d, 2, mring, pb[0])
    S1 = P.sb([128, KC, 2], F32, "S1")
    P.op("dve", lambda e: e.tensor_scalar(out=S1[:], in0=mods[:, 1, :, :], scalar1=1.0, scalar2=None, op0=ALU.add), rd=[mods], wr=[S1])
    X = P.sb([128, KC, 512], F32, "X")
    H = P.sb([128, KC, 512], BF16, "H")

    def load_block(c0, T):
        v = 0 if c0 < 256 else 1
        P.dma("sp", X[:, :, :T], xs_d[:, c0:c0 + T].rearrange("(k p) t -> p k t", p=128), wr=[X])
        for k in range(KC):
            P.op("act", lambda e: e.activation(out=H[:, k, :T], in_=X[:, k, :T], func=AF.Identity,
                                               bias=mods[:, 0, k, v:v + 1], scale=S1[:, k, v:v + 1]), rd=[X, mods, S1], wr=[H])

    def proj(slot, csl, pp, T):
        for k in range(KC):
            P.op("pe", lambda e: e.matmul(pp[:, :T], lhsT=slot[:, k, csl], rhs=H[:, k, :T], start=(k == 0), stop=(k == KC - 1)),
                 rd=[slot, H], wr=[pp])

    if kind == 0:
        w_d = P.din("w3", [16, 128, KC * 384], F32)
        cw_d = P.din("cw", [128, KC, 5], F32)
        oA_d = P.dout("oA", [D, LSEQ], BF16)
        cw = P.sb([128, KC, 5], F32, "cw"); P.dma("sp", cw[:], cw_d[:, :, :], wr=[cw])
        ring = [P.sb([128, KC, 384], BF16, "wr%d" % i) for i in range(3)]
        ws = WStream(P, ring, [w_d[k].rearrange("p (k c) -> p k c", k=KC) for _ in SEQ for k in range(KC)])
        vS = P.sb([128, 512], F32, "vS"); pS = P.sb([128, 512], F32, "pS"); yS = P.sb([128, 512], F32, "yS")
        ob = P.sb([128, KC, 512], BF16, "ob")
        for (c0, T) in SEQ:
            L = 256 if c0 == 0 else 64
            load_block(c0, T)
            for k in range(KC):
                slot = ws.next()
                pbg, pcg, pv = pb[(k % 2) * 3], pb[(k % 2) * 3 + 1], pb[(k % 2) * 3 + 2]
                proj(slot, slice(0, 128), pbg, T)
                proj(slot, slice(128, 256), pcg, T)
                proj(slot, slice(256, 384), pv, T)
                P.op("act", lambda e: e.copy(out=vS[:, :T], in_=pv[:, :T]), rd=[pv], wr=[vS])
                P.op("dve", lambda e: e.tensor_tensor(out=pS[:, :T], in0=pcg[:, :T], in1=vS[:, :T], op=ALU.mult), rd=[pcg, vS], wr=[pS])
                P.op("dve", lambda e: e.tensor_scalar(out=yS[:, :T], in0=pS[:, :T], scalar1=cw[:, k, 2:3], scalar2=None, op0=ALU.mult),
                     rd=[pS, cw], wr=[yS])
                conv_taps(P, yS, pS, cw, k, T, L)
                P.op("dve", lambda e: e.tensor_tensor(out=ob[:, k, :T], in0=pbg[:, :T], in1=yS[:, :T], op=ALU.mult), rd=[pbg, yS], wr=[ob])
            P.dma("sp", oA_d[:, c0:c0 + T].rearrange("(k p) t -> p k t", p=128), ob[:, :, :T], rd=[ob])

    elif kind == 2:
        w_d = P.din("w2", [16, 128, KC * 256], F32)
        cw_d = P.din("cw", [128, KC, 5], F32)
        cb_d = P.din("cb", [128, KC], F32)
        wg_d = P.din("wg", [128, 2 * 8 * 2 * 256], F32)
        bg_d = P.din("bg", [128, 2, KC], F32)
        lam_d = P.din("lam", [128, KC], F32)
        hh_d = P.dout("hh", [D, LSEQ], F32)
        gy_d = P.dout("gy", [D, LSEQ], F32)
        cw = P.sb([128, KC, 5], F32, "cw"); P.dma("sp", cw[:], cw_d[:, :, :], wr=[cw])
        cb = P.sb([128, KC], F32, "cb"); P.dma("sp", cb[:], cb_d[:, :], wr=[cb])
        wg = P.sb([128, 2, 8, 2, 256], BF16, "wg")
        P.dma("pool", wg[:], wg_d.rearrange("p (g n i o) -> p g n i o", g=2, n=8, i=2), wr=[wg])
        bg_ = P.sb([128, 2, KC], F32, "bg_"); P.dma("sp", bg_[:], bg_d[:, :, :], wr=[bg_])
        lam = P.sb([128, KC], F32, "lam"); P.dma("sp", lam[:], lam_d[:, :], wr=[lam])
        c8 = P.sb([128, KC], F32, "c8"); c16 = P.sb([128, KC], F32, "c16")
        P.op("act", lambda e: e.activation(out=c8[:], in_=lam[:], func=AF.Sigmoid), rd=[lam], wr=[c8])
        P.op("act", lambda e: e.activation(out=c8[:], in_=c8[:], func=AF.Ln), rd=[c8], wr=[c8])
        P.op("dve", lambda e: e.tensor_scalar(out=c16[:], in0=c8[:], scalar1=16.0, scalar2=None, op0=ALU.mult), rd=[c8], wr=[c16])
        P.op("dve", lambda e: e.tensor_scalar(out=c8[:], in0=c8[:], scalar1=8.0, scalar2=None, op0=ALU.mult), rd=[c8], wr=[c8])
        hprev = P.sb([128, KC], F32, "hprev")
        P.op("dve", lambda e: e.memset(hprev[:], 0.0), wr=[hprev])
        ring = [P.sb([128, KC, 256], BF16, "wr%d" % i) for i in range(3)]
        ws = WStream(P, ring, [w_d[k].rearrange("p (k c) -> p k c", k=KC) for _ in SEQ for k in range(KC)])
        xc = P.sb([128, KC, 512], F32, "xc"); xcb = P.sb([128, KC, 512], BF16, "xcb")
        gyt = [P.sb([128, 512], F32, "gyt%d" % i) for i in range(2)]; ht = [P.sb([128, 512], F32, "ht%d" % i) for i in range(2)]
        pS = P.sb([128, 512], F32, "pS"); q1 = P.sb([128, 512], F32, "q1"); q2 = P.sb([128, 512], F32, "q2")
        gr = P.sb([128, 512], F32, "gr"); gi = P.sb([128, 512], F32, "gi"); aa = P.sb([128, 512], F32, "aa"); uu = P.sb([128, 512], F32, "uu")
        for (c0, T) in SEQ:
            L = 256 if c0 == 0 else 64
            load_block(c0, T)
            for k in range(KC):
                slot = ws.next()
                py, px = pb[(k % 2) * 2], pb[(k % 2) * 2 + 1]
                proj(slot, slice(0, 128), py, T)
                proj(slot, slice(128, 256), px, T)
                P.op("act", lambda e: e.activation(out=q1[:, :T], in_=py[:, :T], func=AF.Square), rd=[py], wr=[q1])
                P.op("dve", lambda e: e.tensor_scalar(out=q1[:, :T], in0=q1[:, :T], scalar1=0.044715, scalar2=1.0, op0=ALU.mult, op1=ALU.add),
                     rd=[q1], wr=[q1])
                P.op("dve", lambda e: e.tensor_tensor(out=q1[:, :T], in0=py[:, :T], in1=q1[:, :T], op=ALU.mult), rd=[py, q1], wr=[q1])
                P.op("act", lambda e: e.activation(out=q2[:, :T], in_=q1[:, :T], func=AF.Sigmoid, scale=1.5957691216057308), rd=[q1], wr=[q2])
                gq = gyt[k % 2]
                P.op("dve", lambda e: e.tensor_tensor(out=gq[:, :T], in0=py[:, :T], in1=q2[:, :T], op=ALU.mult), rd=[py, q2], wr=[gq])
                P.dma("sp", gy_d[k * 128:(k + 1) * 128, c0:c0 + T], gq[:, :T], rd=[gq])
                P.op("act", lambda e: e.copy(out=pS[:, :T], in_=px[:, :T]), rd=[px], wr=[pS])
                P.op("dve", lambda e: e.tensor_scalar(out=xc[:, k, :T], in0=pS[:, :T], scalar1=cw[:, k, 2:3], scalar2=cb[:, k:k + 1],
                                                      op0=ALU.mult, op1=ALU.add), rd=[pS, cw, cb], wr=[xc])
                y3 = xc[:, k, :T].rearrange("p (r l) -> p r l", l=L)
                p3 = pS[:, :T].rearrange("p (r l) -> p r l", l=L)
                for o in (-2, -1, 1, 2):
                    a, b = max(0, -o), L - max(0, o)
                    P.op("dve", lambda e: e.scalar_tensor_tensor(out=y3[:, :, a:b], in0=p3[:, :, a + o:b + o], scalar=cw[:, k, o + 2:o + 3],
                                                                 in1=y3[:, :, a:b], op0=ALU.mult, op1=ALU.add), rd=[pS, cw, xc], wr=[xc])
                P.op("pool", lambda e: e.tensor_copy(out=xcb[:, k, :T], in_=xc[:, k, :T]), rd=[xc], wr=[xcb])
            for n in range(8):
                for oc in range(2):
                    ch = n * 2 + oc
                    pr_, pi_ = pb[4 + (ch % 2) * 2], pb[5 + (ch % 2) * 2]
                    for g, pp in ((0, pr_), (1, pi_)):
                        for ic in range(2):
                            P.op("pe", lambda e: e.matmul(pp[:, :T], lhsT=wg[:, g, n, ic, oc * 128:(oc + 1) * 128], rhs=xcb[:, n * 2 + ic, :T],
                                                           start=(ic == 0), stop=(ic == 1)), rd=[wg, xcb], wr=[pp])
                    P.op("act", lambda e: e.activation(out=gr[:, :T], in_=pr_[:, :T], func=AF.Sigmoid, bias=bg_[:, 0, ch:ch + 1]), rd=[pr_, bg_], wr=[gr])
                    P.op("act", lambda e: e.activation(out=gi[:, :T], in_=pi_[:, :T], func=AF.Sigmoid, bias=bg_[:, 1, ch:ch + 1]), rd=[pi_, bg_], wr=[gi])
                    P.op("act", lambda e: e.activation(out=aa[:, :T], in_=gr[:, :T], func=AF.Exp, scale=c8[:, ch:ch + 1]), rd=[gr, c8], wr=[aa])
                    P.op("act", lambda e: e.activation(out=uu[:, :T], in_=gr[:, :T], func=AF.Exp, scale=c16[:, ch:ch + 1]), rd=[gr, c16], wr=[uu])
                    P.op("dve", lambda e: e.tensor_scalar(out=uu[:, :T], in0=uu[:, :T], scalar1=-1.0, scalar2=1.0, op0=ALU.mult, op1=ALU.add),
                         rd=[uu], wr=[uu])
                    P.op("act", lambda e: e.activation(out=uu[:, :T], in_=uu[:, :T], func=AF.Sqrt), rd=[uu], wr=[uu])
                    P.op("dve", lambda e: e.tensor_tensor(out=uu[:, :T], in0=uu[:, :T], in1=gi[:, :T], op=ALU.mult), rd=[uu, gi], wr=[uu])
                    P.op("dve", lambda e: e.tensor_tensor(out=uu[:, :T], in0=uu[:, :T], in1=xc[:, ch, :T], op=ALU.mult), rd=[uu, xc], wr=[uu])
                    hq = ht[ch % 2]
                    P.op("dve", lambda e: e.tensor_tensor_scan(out=hq[:, :T], data0=aa[:, :T], data1=uu[:, :T], initial=hprev[:, ch:ch + 1],
                                                               op0=ALU.mult, op1=ALU.add), rd=[aa, uu, hprev], wr=[hq])
                    P.op("act", lambda e: e.copy(out=hprev[:, ch:ch + 1], in_=hq[:, T - 1:T]), rd=[hq], wr=[hprev])
                    P.dma("sp", hh_d[ch * 128:(ch + 1) * 128, c0:c0 + T], hq[:, :T], rd=[hq])

    else:
        nh, ck, cvn = (4, 2, 4) if kind == 1 else (16, 1, 1)
        ncol = 48 if kind == 1 else 64
        w_d = P.din("w1", [ncol, 128, KC * 128], F32)
        bigsel_d = P.din("bigsel", [128, 128 * 128], BF16)
        w2c_d = P.din("w2c", [128, 255], BF16)
        of_d = P.dout("of", [D, LSEQ], F32)
        gg_d = P.dout("gg", [D, LSEQ], F32)
        bigsel = P.sb([128, 128 * 128], BF16, "bigsel"); P.dma("sp", bigsel[:], bigsel_d[:, :], wr=[bigsel])
        w2c = P.sb([128, 255], BF16, "w2c"); P.dma("sp", w2c[:], w2c_d[:, :], wr=[w2c])
        if kind == 1:
            wrk_d = P.din("wrk", [128, KC * 16], F32)
            wg2_d = P.din("wg2", [16, 1024], F32)
            bg2_d = P.din("bg2", [128, 8], F32)
            wrk = P.sb([128, KC, 16], BF16, "wrk"); P.dma("pool", wrk[:], wrk_d.rearrange("p (k c) -> p k c", k=KC), wr=[wrk])
            wg2 = P.sb([16, 1024], BF16, "wg2"); P.dma("pool", wg2[:], wg2_d[:, :], wr=[wg2])
            bg2 = P.sb([128, 8], F32, "bg2"); P.dma("sp", bg2[:], bg2_d[:, :], wr=[bg2])
            rT = P.sb([16, 512], BF16, "rT")
        else:
            lbr_d = P.din("lbr", [128, 4, KC], F32)
            lbr = P.sb([128, 4, KC], F32, "lbr"); P.dma("sp", lbr[:], lbr_d[:, :, :], wr=[lbr])
            el = P.sb([128, 4, KC], F32, "el")
            P.op("act", lambda e: e.activation(out=el[:], in_=lbr[:], func=AF.Exp), rd=[lbr], wr=[el])
            lb = P.sb([128, KC], F32, "lb"); oml = P.sb([128, KC], F32, "oml"); stot = P.sb([128, KC], F32, "stot")
            P.op("dve", lambda e: e.tensor_tensor(out=lb[:], in0=el[:, 1, :], in1=el[:, 2, :], op=ALU.add), rd=[el], wr=[lb])
            P.op("dve", lambda e: e.tensor_tensor(out=lb[:], in0=lb[:], in1=el[:, 3, :], op=ALU.add), rd=[lb, el], wr=[lb])
            P.op("dve", lambda e: e.tensor_tensor(out=stot[:], in0=lb[:], in1=el[:, 0, :], op=ALU.add), rd=[lb, el], wr=[stot])
            P.op("dve", lambda e: e.reciprocal(out=stot[:], in_=stot[:]), rd=[stot], wr=[stot])
            P.op("dve", lambda e: e.tensor_tensor(out=lb[:], in0=lb[:], in1=stot[:], op=ALU.mult), rd=[lb, stot], wr=[lb])
            P.op("dve", lambda e: e.tensor_scalar(out=oml[:], in0=lb[:], scalar1=-1.0, scalar2=1.0, op0=ALU.mult, op1=ALU.add), rd=[lb], wr=[oml])
        ring = [P.sb([128, KC, 128], BF16, "wr%d" % i) for i in range(6)]
        order = []
        for h in range(nh):
            if kind == 1:
                order += [h * 2 + c for c in range(2)] + [8 + h * 2 + c for c in range(2)] + [16 + h * 4 + c for c in range(4)] \
                    + [32 + h * 4 + c for c in range(4)]
            else:
                order += [h, 16 + h, 32 + h, 48 + h]
        ws = WStream(P, ring, [w_d[c].rearrange("p (k c) -> p k c", k=KC) for _ in SEQ for c in order])
        qT = P.sb([128, ck, 512], F32, "qT"); kT = P.sb([128, ck, 512], F32, "kT"); aT = P.sb([128, ck, 512], F32, "aT")
        vT = P.sb([128, cvn, 512], BF16, "vT")
        gob = P.sb([128, 512], F32, "gob"); oob = P.sb([128, 512], F32, "oob")
        ku = [P.sb([128, 512], F32, "ku%d" % i) for i in range(2)]
        Ss = [P.sb([128, 512], F32, "Ss%d" % i) for i in range(2)]
        pr = [P.sb([128, 512], BF16, "pr%d" % i) for i in range(2)]
        Sfin = P.sb([128, nh * ck * cvn * 128], F32, "Sfin")
        P.op("dve", lambda e: e.memset(Sfin[:], 0.0), wr=[Sfin])
        tmp = P.sb([128, 512], F32, "tmp")
        qscale = float(256 ** -0.5) if kind == 1 else float(128 ** -0.5)
        cnt = 0
        for (c0, T) in SEQ:
            load_block(c0, T)
            if kind == 1:
                pr_ = pb[7]
                for k in range(KC):
                    P.op("pe", lambda e: e.matmul(pr_[0:16, :T], lhsT=wrk[:, k, :], rhs=H[:, k, :T], start=(k == 0), stop=(k == KC - 1)),
                         rd=[wrk, H], wr=[pr_])
                P.op("act", lambda e: e.copy(out=rT[:, :T], in_=pr_[0:16, :T]), rd=[pr_], wr=[rT])
            for h in range(nh):
                pq = pb[0]
                if kind == 1:
                    for c in range(ck):
                        proj(ws.next(), slice(0, 128), pq, T)
                        P.op("act", lambda e: e.mul(out=qT[:, c, :T], in_=pq[:, :T], mul=qscale), rd=[pq], wr=[qT])
                    for c in range(ck):
                        proj(ws.next(), slice(0, 128), pq, T)
                        P.op("act", lambda e: e.copy(out=kT[:, c, :T], in_=pq[:, :T]), rd=[pq], wr=[kT])
                    for c in range(cvn):
                        proj(ws.next(), slice(0, 128), pq, T)
                        P.op("act", lambda e: e.copy(out=vT[:, c, :T], in_=pq[:, :T]), rd=[pq], wr=[vT])
                    for c in range(cvn):
                        proj(ws.next(), slice(0, 128), pq, T)
                        P.op("act", lambda e: e.copy(out=gob[:, :T], in_=pq[:, :T]), rd=[pq], wr=[gob])
                        ch = h * cvn + c
                        P.dma("sp", gg_d[ch * 128:(ch + 1) * 128, c0:c0 + T], gob[:, :T], rd=[gob])
                    for c in range(ck):
                        col = h * 256 + c * 128
                        P.op("pe", lambda e: e.matmul(pq[:, :T], lhsT=wg2[:, col:col + 128], rhs=rT[:, :T], start=True, stop=True),
                             rd=[wg2, rT], wr=[pq])
                        P.op("act", lambda e: e.activation(out=aT[:, c, :T], in_=pq[:, :T], func=AF.Sigmoid, bias=bg2[:, h * 2 + c:h * 2 + c + 1]),
                             rd=[pq, bg2], wr=[aT])
                        P.op("act", lambda e: e.activation(out=aT[:, c, :T], in_=aT[:, c, :T], func=AF.Ln), rd=[aT], wr=[aT])
                        P.op("act", lambda e: e.activation(out=aT[:, c, :T], in_=aT[:, c, :T], func=AF.Exp, scale=1.0 / 16.0), rd=[aT], wr=[aT])
                else:
                    proj(ws.next(), slice(0, 128), pq, T)
                    P.op("act", lambda e: e.activation(out=qT[:, 0, :T], in_=pq[:, :T], func=AF.Silu), rd=[pq], wr=[qT])
                    P.op("dve", lambda e: e.tensor_scalar(out=qT[:, 0, :T], in0=qT[:, 0, :T], scalar1=qscale, scalar2=None, op0=ALU.mult),
                         rd=[qT], wr=[qT])
                    proj(ws.next(), slice(0, 128), pq, T)
                    P.op("act", lambda e: e.activation(out=tmp[:, :T], in_=pq[:, :T], func=AF.Sigmoid), rd=[pq], wr=[tmp])
                    P.op("dve", lambda e: e.tensor_scalar(out=aT[:, 0, :T], in0=tmp[:, :T], scalar1=oml[:, h:h + 1], scalar2=lb[:, h:h + 1],
                                                          op0=ALU.mult, op1=ALU.add), rd=[tmp, oml, lb], wr=[aT])
                    P.op("dve", lambda e: e.tensor_scalar(out=kT[:, 0, :T], in0=aT[:, 0, :T], scalar1=-1.0, scalar2=1.0, op0=ALU.mult, op1=ALU.add),
                         rd=[aT], wr=[kT])
                    proj(ws.next(), slice(0, 128), pq, T)
                    P.op("act", lambda e: e.copy(out=vT[:, 0, :T], in_=pq[:, :T]), rd=[pq], wr=[vT])
                    proj(ws.next(), slice(0, 128), pq, T)
                    P.op("act", lambda e: e.copy(out=gob[:, :T], in_=pq[:, :T]), rd=[pq], wr=[gob])
                    P.dma("sp", gg_d[h * 128:(h + 1) * 128, c0:c0 + T], gob[:, :T], rd=[gob])
                for cvi in range(cvn):
                    po = pb[1 + (cvi % 2)]
                    for jj in range(128):
                        pv_ = pb[3 + (jj % 2)]
                        P.op("pe", lambda e: e.matmul(pv_[:, :T], lhsT=bigsel[:, jj * 128:(jj + 1) * 128], rhs=vT[:, cvi, :T], start=True, stop=True),
                             rd=[bigsel, vT], wr=[pv_])
                        for c in range(ck):
                            b2 = cnt % 2
                            cnt += 1
                            sidx = ((h * ck + c) * cvn + cvi) * 128 + jj
                            P.op("dve", lambda e: e.tensor_tensor(out=ku[b2][:, :T], in0=kT[:, c, :T], in1=pv_[:, :T], op=ALU.mult),
                                 rd=[kT, pv_], wr=[ku[b2]])
                            P.op("dve", lambda e: e.tensor_tensor_scan(out=Ss[b2][:, :T], data0=aT[:, c, :T], data1=ku[b2][:, :T],
                                                                       initial=Sfin[:, sidx:sidx + 1], op0=ALU.mult, op1=ALU.add),
                                 rd=[aT, ku[b2], Sfin], wr=[Ss[b2]])
                            P.op("act", lambda e: e.copy(out=Sfin[:, sidx:sidx + 1], in_=Ss[b2][:, T - 1:T]), rd=[Ss[b2]], wr=[Sfin])
                            P.op("pool", lambda e: e.tensor_tensor(out=pr[b2][:, :T], in0=qT[:, c, :T], in1=Ss[b2][:, :T], op=ALU.mult),
                                 rd=[qT, Ss[b2]], wr=[pr[b2]])
                            P.op("pe", lambda e: e.matmul(po[:, :T], lhsT=w2c[:, 127 - jj:255 - jj], rhs=pr[b2][:, :T],
                                                           start=(jj == 0 and c == 0), stop=(jj == 127 and c == ck - 1)),
                                 rd=[w2c, pr[b2]], wr=[po])
                    P.op("act", lambda e: e.copy(out=oob[:, :T], in_=po[:, :T]), rd=[po], wr=[oob])
                    ch = h * cvn + cvi
                    P.dma("sp", of_d[ch * 128:(ch + 1) * 128, c0:c0 + T], oob[:, :T], rd=[oob])
    return P.finish()


def _bf(a):
    return a.astype(ml_dtypes.bfloat16)


def _taps5(w, bwd):
    K_ = w.shape[0]
    t = np.zeros((5, D), np.float32)
    for j in range(K_):
        o = j - 1
        t[(-o if bwd else o) + 2] = w[j]
    return np.ascontiguousarray(vec_pk(t).transpose(0, 2, 1))


def _unrev(a):
    return np.concatenate([a[..., :256][..., ::-1], a[..., 256:][..., ::-1]], axis=-1)


_CONST = {}


def _consts():
    if not _CONST:
        bs = np.zeros((128, 128, 128), np.float32)
        for j in range(128):
            bs[j, j, :] = 1.0
        _CONST["bigsel"] = _bf(bs.reshape(128, 128 * 128))
        w2c = np.zeros((128, 255), np.float32)
        w2c[:, 127] = 1.0
        _CONST["w2c"] = _bf(w2c)
        _CONST["sel"] = make_sel()
    return _CONST


def phase_a_weights(kind, i, inp, d):
    m = {}
    m["adaw"] = relayout_kn(inp["ada_w"][i][:, 0:2 * D])
    m["adab"] = vec_pk(inp["ada_b"][i][:2 * D].reshape(2, D))
    if kind == 0:
        W3 = np.ascontiguousarray(inp["sc_w_in"][0].reshape(D, 3, 16, 128).transpose(0, 2, 1, 3)).reshape(D, 3 * D)
        m["w3"] = relayout_kn(W3, cols=384)
        m["cw"] = _taps5(inp["sc_conv"][0], d == 1)
    elif kind == 2:
        W2 = np.ascontiguousarray(inp["lru_w_in"][0].reshape(D, 2, 16, 128).transpose(0, 2, 1, 3)).reshape(D, 2 * D)
        m["w2"] = relayout_kn(W2, cols=256)
        m["cw"] = _taps5(inp["lru_conv"][0], d == 1)
        m["cb"] = vec_pk(inp["lru_conv_b"][0])
        wg = inp["lru_w_gate"][0][d]
        m["wg"] = np.ascontiguousarray(wg.reshape(2, 8, 2, 128, 256).transpose(3, 0, 1, 2, 4)).reshape(128, 2 * 8 * 2 * 256)
        m["bg"] = vec_pk(inp["lru_b_gate"][0][d])
        m["lam"] = vec_pk(inp["lru_lambda"][0][d])
    elif kind == 1:
        w = inp["gla_w_in"][0]
        m["w1"] = relayout_kn(np.ascontiguousarray(w[:, :6144]), cols=128)
        rk = w[:, 6144 + d * 16:6144 + (d + 1) * 16]
        m["wrk"] = np.ascontiguousarray(rk.reshape(16, 128, 16).transpose(1, 0, 2)).reshape(128, 256)
        m["wg2"] = np.ascontiguousarray(inp["gla_w_gate2"][0][d])
        m["bg2"] = np.ascontiguousarray(inp["gla_b_gate"][0][d].reshape(8, 128).T)
        m["bigsel"] = _consts()["bigsel"]; m["w2c"] = _consts()["w2c"]
    else:
        w = inp["hg_w_in"][0]
        f0 = 2048 + d * 2048
        W = np.concatenate([w[:, 0:2048], w[:, f0:f0 + 2048], w[:, 6144:8192], w[:, 8192:10240]], axis=1)
        m["w1"] = relayout_kn(W, cols=128)
        m["lbr"] = vec_pk(inp["hg_lb_raw"])
        m["bigsel"] = _consts()["bigsel"]; m["w2c"] = _consts()["w2c"]
    return m


def phase_b_weights(kind, i, inp):
    m = {}
    m["adaw"] = relayout_kn(inp["ada_w"][i][:, 2 * D:6 * D])
    m["adab"] = vec_pk(inp["ada_b"][i][2 * D:].reshape(4, D))
    m["lng"] = vec_pk(inp["ln_g"][i]); m["lnb"] = vec_pk(inp["ln_b"][i])
    wo = [inp["sc_w_out"], inp["gla_w_out"], inp["lru_w_out"], inp["hg_w_out"]][kind][0]
    m["wo"] = relayout_kn(wo)
    m["wr"] = np.ascontiguousarray(inp["moe_w_router"][i].reshape(16, 128, 32).transpose(1, 0, 2))
    m["br"] = np.ascontiguousarray(np.broadcast_to(inp["moe_b_router"][i][None, :], (128, 32)))
    m["wgu"] = relayout_gu(inp["moe_w_gu"][i])
    m["wd"] = np.ascontiguousarray(inp["moe_w_down"][i])
    m["bgu"] = np.ascontiguousarray(inp["moe_b_gu"][i].reshape(32, 12, 128).transpose(2, 0, 1))
    m["bdn"] = np.ascontiguousarray(inp["moe_b_down"][i])
    m["sel"] = _consts()["sel"]
    if kind == 1:
        m["gain"] = np.ascontiguousarray(inp["gla_norm"][0].reshape(4, 128).T)
    elif kind == 3:
        m["gain"] = np.ascontiguousarray(inp["hg_norm"][0].reshape(1, 128).T)
    return m


_PROGS = {}


def _prog(key, fn):
    if key not in _PROGS:
        _PROGS[key] = fn()
    return _PROGS[key]


def run_phase_a(kind, i, inp, xT_all):
    wts = [phase_a_weights(kind, i, inp, d) for d in range(2)]
    in_maps = []
    for b in range(4):
        xb = xT_all[:, b * LSEQ:(b + 1) * LSEQ]
        cv = np.ascontiguousarray(np.stack([vec_pk(inp["c_ctx"]), vec_pk(inp["c"][b])], axis=-1))
        for d in range(2):
            xs = xb if d == 0 else np.concatenate([xb[:, :256][:, ::-1], xb[:, 256:][:, ::-1]], axis=1)
            m = dict(wts[d])
            m["xs"] = np.ascontiguousarray(xs)
            m["cv"] = cv
            in_maps.append(m)
    nc = _prog(("a", kind), lambda: build_phase_a(kind))
    res = run_bass_kernel_spmd(nc, in_maps, core_ids=list(range(8)))
    return res.results


def run_phase_b(kind, i, inp, xT_all, ares):
    wts = phase_b_weights(kind, i, inp)
    in_maps = []
    for b in range(4):
        rf, rb = ares[2 * b], ares[2 * b + 1]
        for half in range(2):
            sl = slice(half * NTOK, (half + 1) * NTOK)
            m = dict(wts)
            m["xT"] = np.ascontiguousarray(xT_all[:, b * LSEQ:(b + 1) * LSEQ][:, sl])
            cA = inp["c_ctx"] if half == 0 else inp["c"][b]
            m["cv"] = np.ascontiguousarray(np.stack([vec_pk(cA), vec_pk(inp["c"][b])], axis=-1))
            if kind == 0:
                m["oT"] = np.ascontiguousarray(rf["oA"][:, sl])
            else:
                fo, go = ("hh", "gy") if kind == 2 else ("of", "gg")
                m["oF"] = np.ascontiguousarray(rf[fo][:, sl])
                m["oB"] = np.ascontiguousarray(_unrev(rb[fo])[:, sl])
                m["gG"] = np.ascontiguousarray(rf[go][:, sl])
            in_maps.append(m)
    nc = _prog(("b", kind), lambda: build_phase_b(kind))
    res = run_bass_kernel_spmd(nc, in_maps, core_ids=list(range(8)))
    out = np.empty_like(xT_all)
    for b in range(4):
        for half in range(2):
            out[:, b * LSEQ + half * NTOK:b * LSEQ + (half + 1) * NTOK] = res.results[2 * b + half]["xo"]
    return out


def kernel(**inp):
    inp = {k: np.asarray(v) for k, v in inp.items()}
    x, ctx = inp["x"], inp["ctx"]
    xT_all = np.empty((D, 4 * LSEQ), np.float32)
    for b in range(4):
        xT_all[:, b * LSEQ:b * LSEQ + 256] = ctx[b].T
        xT_all[:, b * LSEQ + 256:(b + 1) * LSEQ] = x[b].T
    for i in range(DEPTH):
        kind = i % 4
        ares = run_phase_a(kind, i, inp, xT_all)
        xT_all = run_phase_b(kind, i, inp, xT_all, ares)
    out = np.empty((4, 4096, D), np.float32)
    for b in range(4):
        out[b] = xT_all[:, b * LSEQ + 256:(b + 1) * LSEQ].T
    return out
```
